# Optimizing a Trainium2 kernel written in Bass

```python
import math
import jax
import jax.numpy as jnp
from jax import lax
import numpy as np

D_MODEL = 2048
BATCH = 1
SEQ = 16384
DEPTH = 2

GRID_W = 64
Q_BLOCK = 128
HEAD_DIM = 128
NORM_EPS = 1e-6

A_HEADS = 4
A_QK_DIM = HEAD_DIM // 2
A_V_DIM = HEAD_DIM
A_WIDTH = A_HEADS * A_V_DIM
B_HEADS = 8
B_KV_HEADS = 2
B_WIDTH = B_HEADS * HEAD_DIM
ROPE_THETA = 10000.0
C_HEADS = 4
C_WIDTH = C_HEADS * HEAD_DIM
NA_ROWS = 8
NA_COLS = 16
T5_BUCKETS = 32
T5_MAX_DIST = 128
N_BRANCH = 3
SPLIT_SIZES = (A_HEADS * 2 * A_QK_DIM, A_HEADS * 2 * A_QK_DIM, A_WIDTH,
               B_WIDTH, B_KV_HEADS * HEAD_DIM, B_KV_HEADS * HEAD_DIM,
               C_WIDTH, C_WIDTH, C_WIDTH,
               N_BRANCH * D_MODEL)
IN_WIDTH = 10752
FFN_DIM = 5632
N_EXPERTS = 8
TOP_K = 2
EXPERT_DIM = 7168
MOE_BLOCK = 128
N_DENSE = (DEPTH + 1) // 2
N_MOE = DEPTH // 2

kernel_name = "hybrid_diffattn_gqa_natten_moe_encoder"


def _rmsnorm(x, g):
    xf = x.astype(jnp.float32)
    y = xf * lax.rsqrt(jnp.mean(xf * xf, axis=-1, keepdims=True) + NORM_EPS)
    return (y * g.astype(jnp.float32)).astype(x.dtype)


def _t5_bucket(rel):
    nb = T5_BUCKETS // 2
    max_exact = nb // 2
    ret = jnp.where(rel > 0, nb, 0)
    n = jnp.abs(rel)
    nf = jnp.maximum(n, 1).astype(jnp.float32)
    large = max_exact + (jnp.log(nf / max_exact) / math.log(T5_MAX_DIST / max_exact)
                         * (nb - max_exact)).astype(jnp.int32)
    large = jnp.minimum(large, nb - 1)
    return ret + jnp.where(n < max_exact, n, large)


def _diff_attention(q, k, v, lam, lam_init, subln_g, t5_table):
    bn, s_len = q.shape[0], q.shape[1]
    nblk = s_len // Q_BLOCK
    scale = A_QK_DIM ** -0.5
    qh = jnp.transpose(q, (0, 2, 3, 1, 4))
    kh = jnp.transpose(k, (0, 2, 3, 1, 4))
    vh = jnp.transpose(v, (0, 2, 1, 3))
    qblk = jnp.moveaxis(qh.reshape(bn, A_HEADS, 2, nblk, Q_BLOCK, A_QK_DIM), 3, 0)
    kpos = jnp.arange(s_len, dtype=jnp.int32)

    def block(args):
        qb, i = args
        qpos = i * Q_BLOCK + jnp.arange(Q_BLOCK, dtype=jnp.int32)
        bias = t5_table[_t5_bucket(kpos[None, :] - qpos[:, None])]
        bias = jnp.transpose(bias, (2, 0, 1)).astype(jnp.float32)
        sc = jnp.einsum('bhmqd,bhmkd->bhmqk', qb, kh).astype(jnp.float32) * scale
        p = jax.nn.softmax(sc + bias[None, :, None], axis=-1)
        w = (p[:, :, 0] - lam * p[:, :, 1]).astype(vh.dtype)
        return jnp.einsum('bhqk,bhkd->bhqd', w, vh)

    o = lax.map(block, (qblk, jnp.arange(nblk, dtype=jnp.int32)))
    o = jnp.transpose(o, (1, 0, 3, 2, 4)).reshape(bn, s_len, A_HEADS, A_V_DIM)
    o = _rmsnorm(o, subln_g) * (1.0 - lam_init)
    return o.reshape(bn, s_len, A_WIDTH)


def _rope_segment(xs, ang):
    c = jnp.cos(ang)[None, :, None, :]
    s = jnp.sin(ang)[None, :, None, :]
    x1, x2 = jnp.split(xs, 2, axis=-1)
    return jnp.concatenate([x1 * c - x2 * s, x2 * c + x1 * s], axis=-1)


def _axial_rope(x, row, col):
    half = HEAD_DIM // 2
    inv = ROPE_THETA ** (-jnp.arange(0, half, 2, dtype=jnp.float32) / half)
    ang_r = row.astype(jnp.float32)[:, None] * inv[None, :]
    ang_c = col.astype(jnp.float32)[:, None] * inv[None, :]
    xf = x.astype(jnp.float32)
    out = jnp.concatenate([_rope_segment(xf[..., :half], ang_r),
                           _rope_segment(xf[..., half:], ang_c)], axis=-1)
    return out.astype(x.dtype)


def _gqa_axial(q, k, v, q_gain, k_gain):
    bn, s_len = q.shape[0], q.shape[1]
    nblk = s_len // Q_BLOCK
    group = B_HEADS // B_KV_HEADS
    scale = HEAD_DIM ** -0.5
    pos = jnp.arange(s_len, dtype=jnp.int32)
    row, col = pos // GRID_W, pos % GRID_W
    q = _axial_rope(_rmsnorm(q, q_gain), row, col)
    k = _axial_rope(_rmsnorm(k, k_gain), row, col)
    qh = jnp.transpose(q.reshape(bn, s_len, B_KV_HEADS, group, HEAD_DIM), (0, 2, 3, 1, 4))
    kh = jnp.transpose(k, (0, 2, 1, 3))
    vh = jnp.transpose(v, (0, 2, 1, 3))
    qblk = jnp.moveaxis(qh.reshape(bn, B_KV_HEADS, group, nblk, Q_BLOCK, HEAD_DIM), 3, 0)

    def block(qb):
        sc = jnp.einsum('bkgqd,bksd->bkgqs', qb, kh).astype(jnp.float32) * scale
        p = jax.nn.softmax(sc, axis=-1).astype(vh.dtype)
        return jnp.einsum('bkgqs,bksd->bkgqd', p, vh)

    o = lax.map(block, qblk)
    return jnp.transpose(o, (1, 0, 4, 2, 3, 5)).reshape(bn, s_len, B_WIDTH)


def _neighbourhood_attention(q, k, v, rpb):
    bn, s_len = q.shape[0], q.shape[1]
    rows = s_len // GRID_W
    kh_ = min(NA_ROWS, rows)
    kw = NA_COLS
    scale = HEAD_DIM ** -0.5

    def to_grid(t):
        return jnp.transpose(t.reshape(bn, rows, GRID_W, C_HEADS, HEAD_DIM), (0, 3, 1, 2, 4))

    qg, kg, vg = to_grid(q), to_grid(k), to_grid(v)
    r = jnp.arange(rows, dtype=jnp.int32)
    rs = jnp.clip(r - kh_ // 2, 0, rows - kh_)
    ridx = rs[:, None] + jnp.arange(kh_, dtype=jnp.int32)[None, :]
    c = jnp.arange(GRID_W, dtype=jnp.int32)
    cs = jnp.clip(c - kw // 2, 0, GRID_W - kw)
    valid = (c[None, :] >= cs[:, None]) & (c[None, :] < cs[:, None] + kw)
    kb = kg[:, :, ridx]
    vb = vg[:, :, ridx]
    sc = jnp.einsum('bhrqd,bhrikd->bhrqik', qg, kb).astype(jnp.float32) * scale
    roff = ridx - r[:, None] + (NA_ROWS - 1)
    coff = jnp.clip(c[None, :] - c[:, None] + (NA_COLS - 1), 0, 2 * NA_COLS - 2)
    bias = rpb[:, roff[:, None, :, None], coff[None, :, None, :]]
    sc = sc + bias[None].astype(jnp.float32)
    sc = jnp.where(valid[:, None, :], sc, -1e30)
    p = jax.nn.softmax(sc, axis=(-2, -1)).astype(vb.dtype)
    o = jnp.einsum('bhrqik,bhrikd->bhrqd', p, vb)
    return jnp.transpose(o, (0, 2, 3, 1, 4)).reshape(bn, s_len, C_WIDTH)


def _swiglu(h, w_gate, w_up, w_down):
    return (jax.nn.silu(h @ w_gate) * (h @ w_up)) @ w_down


def _moe_swiglu(h, w_router, w_gate, w_up, w_down):
    bn, s_len, d = h.shape
    n_tok = bn * s_len
    hf = h.reshape(n_tok, d)
    logits = (hf @ w_router).astype(jnp.float32)
    top_v, top_i = lax.top_k(logits, TOP_K)
    gates = jax.nn.softmax(top_v, axis=-1)
    n_assign = n_tok * TOP_K
    e_flat = top_i.reshape(-1).astype(jnp.int32)
    tok_flat = jnp.repeat(jnp.arange(n_tok, dtype=jnp.int32), TOP_K)
    g_flat = gates.reshape(-1)
    order = jnp.argsort(e_flat)
    e_sorted, tok_sorted, g_sorted = e_flat[order], tok_flat[order], g_flat[order]
    counts = jnp.bincount(e_flat, length=N_EXPERTS).astype(jnp.int32)
    starts = jnp.cumsum(counts) - counts
    pcounts = (counts + MOE_BLOCK - 1) // MOE_BLOCK * MOE_BLOCK
    pends = jnp.cumsum(pcounts)
    pstarts = pends - pcounts
    cap = (n_assign + MOE_BLOCK - 1) // MOE_BLOCK * MOE_BLOCK + N_EXPERTS * MOE_BLOCK
    rank = jnp.arange(n_assign, dtype=jnp.int32) - starts[e_sorted]
    dest = pstarts[e_sorted] + rank
    buf_tok = jnp.zeros((cap,), jnp.int32).at[dest].set(tok_sorted)
    buf_g = jnp.zeros((cap,), jnp.float32).at[dest].set(g_sorted)
    nblk = cap // MOE_BLOCK
    blk_e = jnp.minimum(jnp.searchsorted(pends, jnp.arange(nblk, dtype=jnp.int32) * MOE_BLOCK,
                                         side='right'), N_EXPERTS - 1).astype(jnp.int32)
    xb = hf[buf_tok].reshape(nblk, MOE_BLOCK, d)

    def expert_block(args):
        xe, e = args
        return _swiglu(xe, w_gate[e], w_up[e], w_down[e])

    yb = lax.map(expert_block, (xb, blk_e)).reshape(cap, d)
    y = jax.ops.segment_sum(yb * buf_g[:, None].astype(yb.dtype), buf_tok, num_segments=n_tok)
    return y.reshape(bn, s_len, d)


def setup_inputs(seed: int = 0) -> dict:
    key = jax.random.key(seed)
    ks = jax.random.split(key, 24)
    f32 = jnp.float32
    d = D_MODEL

    def nrm(k, shape, scale):
        return jax.random.normal(k, shape, f32) * scale

    return {
        "x": nrm(ks[0], (BATCH, SEQ, d), 1.0),
        "w_in": nrm(ks[1], (DEPTH, d, IN_WIDTH), d ** -0.5),
        "w_branch_a": nrm(ks[2], (DEPTH, A_WIDTH, d), A_WIDTH ** -0.5),
        "w_branch_b": nrm(ks[3], (DEPTH, B_WIDTH, d), B_WIDTH ** -0.5),
        "w_branch_c": nrm(ks[4], (DEPTH, C_WIDTH, d), C_WIDTH ** -0.5),
        "w_out": nrm(ks[5], (DEPTH, d, d), d ** -0.5),
        "norm_mix": 1.0 + nrm(ks[6], (DEPTH, d), 0.02),
        "norm_ffn": 1.0 + nrm(ks[7], (DEPTH, d), 0.02),
        "norm_final": 1.0 + nrm(ks[8], (d,), 0.02),
        "t5_bias": nrm(ks[9], (T5_BUCKETS, A_HEADS), 0.5),
        "diff_lambda": nrm(ks[10], (DEPTH, 4, A_QK_DIM), 0.1),
        "diff_subln": 1.0 + nrm(ks[11], (DEPTH, A_V_DIM), 0.02),
        "qk_norm_b": 1.0 + nrm(ks[12], (DEPTH, 2, HEAD_DIM), 0.02),
        "na_rpb": nrm(ks[13], (DEPTH, C_HEADS, 2 * NA_ROWS - 1, 2 * NA_COLS - 1), 0.5),
        "ffn_gate": nrm(ks[14], (N_DENSE, d, FFN_DIM), d ** -0.5),
        "ffn_up": nrm(ks[15], (N_DENSE, d, FFN_DIM), d ** -0.5),
        "ffn_down": nrm(ks[16], (N_DENSE, FFN_DIM, d), FFN_DIM ** -0.5),
        "moe_router": nrm(ks[17], (N_MOE, d, N_EXPERTS), d ** -0.5),
        "moe_gate": nrm(ks[18], (N_MOE, N_EXPERTS, d, EXPERT_DIM), d ** -0.5),
        "moe_up": nrm(ks[19], (N_MOE, N_EXPERTS, d, EXPERT_DIM), d ** -0.5),
        "moe_down": nrm(ks[20], (N_MOE, N_EXPERTS, EXPERT_DIM, d), EXPERT_DIM ** -0.5),
    }


def reference(x, w_in, w_branch_a, w_branch_b, w_branch_c, w_out, norm_mix, norm_ffn,
              norm_final, t5_bias, diff_lambda, diff_subln, qk_norm_b, na_rpb,
              ffn_gate, ffn_up, ffn_down, moe_router, moe_gate, moe_up, moe_down):
    bn, s_len, d = x.shape
    split_points = [int(v) for v in np.cumsum(SPLIT_SIZES)[:-1]]
    for l in range(DEPTH):
        h = _rmsnorm(x, norm_mix[l])
        z = h @ w_in[l]
        qa, ka, va, qb, kb, vb, qc, kc, vc, gz = jnp.split(z, split_points, axis=-1)
        qa = qa.reshape(bn, s_len, A_HEADS, 2, A_QK_DIM)
        ka = ka.reshape(bn, s_len, A_HEADS, 2, A_QK_DIM)
        va = va.reshape(bn, s_len, A_HEADS, A_V_DIM)
        qb = qb.reshape(bn, s_len, B_HEADS, HEAD_DIM)
        kb = kb.reshape(bn, s_len, B_KV_HEADS, HEAD_DIM)
        vb = vb.reshape(bn, s_len, B_KV_HEADS, HEAD_DIM)
        qc = qc.reshape(bn, s_len, C_HEADS, HEAD_DIM)
        kc = kc.reshape(bn, s_len, C_HEADS, HEAD_DIM)
        vc = vc.reshape(bn, s_len, C_HEADS, HEAD_DIM)
        gates = jax.nn.sigmoid(gz.astype(jnp.float32)).astype(x.dtype).reshape(bn, s_len, N_BRANCH, d)

        lam_init = 0.8 - 0.6 * math.exp(-0.3 * l)
        lp = diff_lambda[l].astype(jnp.float32)
        lam = jnp.exp(jnp.sum(lp[0] * lp[1])) - jnp.exp(jnp.sum(lp[2] * lp[3])) + lam_init

        ya = _diff_attention(qa, ka, va, lam, lam_init, diff_subln[l], t5_bias)
        yb = _gqa_axial(qb, kb, vb, qk_norm_b[l, 0], qk_norm_b[l, 1])
        yc = _neighbourhood_attention(qc, kc, vc, na_rpb[l])

        merged = (gates[:, :, 0] * (ya @ w_branch_a[l])
                  + gates[:, :, 1] * (yb @ w_branch_b[l])
                  + gates[:, :, 2] * (yc @ w_branch_c[l]))
        x = x + merged @ w_out[l]

        h = _rmsnorm(x, norm_ffn[l])
        if l % 2 == 0:
            j = l // 2
            x = x + _swiglu(h, ffn_gate[j], ffn_up[j], ffn_down[j])
        else:
            j = l // 2
            x = x + _moe_swiglu(h, moe_router[j], moe_gate[j], moe_up[j], moe_down[j])
    return _rmsnorm(x, norm_final)
```

```python
import math
import os
import numpy as np
import concourse.bass as bass
import concourse.mybir as mybir
from concourse.bass_utils import run_bass_kernel_spmd

F32 = mybir.dt.float32
BF16 = mybir.dt.bfloat16
I32 = mybir.dt.int32
AF = mybir.ActivationFunctionType
ALU = mybir.AluOpType

NCORES = 8


class Cfg:
    D = 2048
    SEQ = 16384
    DEPTH = 2
    FFN = 5632
    EDIM = 7168
    NE = 8
    GRID_W = 64

    @property
    def TOK(self):
        return self.SEQ // NCORES

    @property
    def ROWS(self):
        return self.SEQ // self.GRID_W


CFG = Cfg()
IN_WIDTH = 10752
EPS = 1e-6


class Op:
    __slots__ = ("eng", "fn", "deps", "is_dma", "key", "needed", "semval", "dmaval", "idx")


class Prog:
    ENGS = ("pe", "act", "dve", "pool", "sp")

    def __init__(self, nc):
        self.nc = nc
        self.ops = {e: [] for e in self.ENGS}
        self.last_w = {}
        self.readers = {}
        self.dma_count = {}
        self.n_ops = 0

    def _tok_deps(self, reads, writes):
        deps = []
        for r in reads:
            w = self.last_w.get(r)
            if w is not None:
                deps.append(w)
        for r in writes:
            w = self.last_w.get(r)
            if w is not None:
                deps.append(w)
            deps.extend(self.readers.get(r, ()))
        return deps

    def _commit(self, op, reads, writes):
        for r in reads:
            self.readers.setdefault(r, []).append(op)
        for r in writes:
            self.last_w[r] = op
            self.readers[r] = []

    def op(self, eng, fn, reads=(), writes=()):
        o = Op()
        o.eng, o.fn, o.is_dma, o.key, o.needed = eng, fn, False, None, False
        o.deps = self._tok_deps(reads, writes)
        o.idx = self.n_ops
        self.n_ops += 1
        self.ops[eng].append(o)
        self._commit(o, reads, writes)
        return o

    def dma(self, eng, fn, key, reads=(), writes=(), inc=16):
        o = Op()
        o.eng, o.fn, o.is_dma, o.key, o.needed = eng, fn, True, key, True
        o.deps = self._tok_deps(reads, writes)
        o.idx = self.n_ops
        self.n_ops += 1
        self.dma_count[key] = self.dma_count.get(key, 0) + inc
        o.dmaval = self.dma_count[key]
        o.semval = inc
        if eng == "pool" and inc == 16:
            pd = self.__dict__.setdefault("pool_dmas", [])
            if len(pd) >= 2:
                o.deps.append(pd[-2])
            pd.append(o)
        self.ops[eng].append(o)
        self._commit(o, reads, writes)
        return o

    def barrier(self):
        deps = []
        for e in self.ENGS:
            lastc = None
            lastk = {}
            for o in self.ops[e]:
                if o.is_dma:
                    lastk[o.key] = o
                else:
                    lastc = o
            if lastc is not None:
                deps.append(lastc)
            deps.extend(lastk.values())
        for e in self.ENGS:
            if e == "sp" or self.ops[e]:
                o = self.op(e, lambda eng: eng.nop())
                o.deps = list(deps)

    def emit(self):
        nc = self.nc
        for e in self.ENGS:
            for o in self.ops[e]:
                for d in o.deps:
                    if not d.is_dma and d is not o:
                        d.needed = True
        EPOCH = 30000
        import contextlib
        stack = contextlib.ExitStack()
        sems = {}

        def get_sem(name):
            if name not in sems:
                sems[name] = stack.enter_context(nc.semaphore(name))
            return sems[name]

        for e in self.ENGS:
            cnt = 0
            ep = 0
            for o in self.ops[e]:
                if o.is_dma:
                    continue
                if o.needed:
                    if cnt == EPOCH:
                        ep += 1
                        cnt = 0
                    cnt += 1
                    o.semval = (f"c_{e}_{ep}", cnt)
        with stack:
            for k in self.dma_count:
                get_sem("d_" + str(k))
            for e in self.ENGS:
                for o in self.ops[e]:
                    if not o.is_dma and o.needed:
                        get_sem(o.semval[0])
            block = stack.enter_context(nc.Block())
            engmap = {"pe": "tensor", "act": "scalar", "dve": "vector", "pool": "gpsimd", "sp": "sync"}

            def make(e):
                def body(eng):
                    waited = {}
                    for o in self.ops[e]:
                        need = {}
                        for d in o.deps:
                            if d.is_dma:
                                nm, v = "d_" + str(d.key), d.dmaval
                            else:
                                nm, v = d.semval
                            if waited.get(nm, 0) >= v:
                                continue
                            if need.get(nm, 0) < v:
                                need[nm] = v
                        for nm, v in need.items():
                            eng.wait_ge(sems[nm], v)
                            waited[nm] = v
                        ins = o.fn(eng)
                        if o.is_dma:
                            ins.then_inc(sems["d_" + str(o.key)], o.semval)
                        elif o.needed:
                            ins.then_inc(sems[o.semval[0]], 1)
                return body

            for e in self.ENGS:
                if self.ops[e]:
                    getattr(block, engmap[e])(make(e))


def _t5_bucket_np(rel):
    nb = 16
    max_exact = 8
    ret = np.where(rel > 0, nb, 0)
    n = np.abs(rel)
    nf = np.maximum(n, 1).astype(np.float32)
    large = max_exact + (np.log(nf / np.float32(max_exact)) / np.float32(math.log(128 / max_exact))
                         * np.float32(nb - max_exact)).astype(np.int32)
    large = np.minimum(large, nb - 1)
    return ret + np.where(n < max_exact, n, large)


def _t5_bucket_exact(rel):
    return _t5_bucket_np(rel)


def host_tables(cfg, core):
    TOK, S, GW, ROWS = cfg.TOK, cfg.SEQ, cfg.GRID_W, cfg.ROWS
    t = core * TOK + np.arange(TOK)
    row, col = t // GW, t % GW
    inv = (10000.0 ** (-np.arange(0, 64, 2, dtype=np.float32) / np.float32(64))).astype(np.float32)
    d = np.arange(128)
    pos = np.where((d // 64)[:, None] == 0, row[None, :], col[None, :]).astype(np.float32)
    ang = pos * inv[d % 32][:, None]
    cosT = np.cos(ang).astype(np.float32)
    sinT = np.sin(ang).astype(np.float32)
    L = TOK + S - 1
    y = np.arange(L)
    rel = -(y - (S - 1)) - core * TOK
    t5idx = _t5_bucket_exact(rel.astype(np.int64)).astype(np.int64)
    RPC = TOK // GW
    KH = min(8, ROWS)
    nkb = KH * GW // 128
    idxC = np.zeros((128, RPC * nkb), np.int32)
    classes = []
    for rl in range(RPC):
        R = core * RPC + rl
        rs = min(max(R - KH // 2, 0), ROWS - KH)
        for kb in range(nkb):
            tglob = rs * GW + kb * 128 + np.arange(128)
            nrc = min(256, TOK)
            rr, rem = tglob // TOK, tglob % TOK
            idxC[:, rl * nkb + kb] = ((rem // nrc) * NCORES + rr) * nrc + rem % nrc
        classes.append((R, rs))
    return cosT, sinT, t5idx, idxC, classes


def na_bias_tile(cfg, rpb_l, R, rs):
    GW, ROWS = cfg.GRID_W, cfg.ROWS
    KH = min(8, ROWS)
    nkb = KH * GW // 128
    c = np.arange(GW)
    cs = np.clip(c - 8, 0, GW - 16)
    valid = (c[None, :] >= cs[:, None]) & (c[None, :] < cs[:, None] + 16)
    coff = np.clip(c[None, :] - c[:, None] + 15, 0, 30)
    out = np.empty((128, 4, nkb, GW), np.float32)
    for kb in range(nkb):
        for half in range(2):
            i = kb * 2 + half
            roff = rs + i - R + 7
            b = rpb_l[:, roff][:, coff]
            b = np.where(valid[None], b, np.float32(-1e30))
            out[half * 64:(half + 1) * 64, :, kb, :] = np.transpose(b, (2, 0, 1))
    return out


def build_program(cfg, debug_stop=None, dbg=None):
    D, S, TOK, FFN, EDIM, NE = cfg.D, cfg.SEQ, cfg.TOK, cfg.FFN, cfg.EDIM, cfg.NE
    DEPTH = cfg.DEPTH
    KC = D // 128
    NTT = TOK // 512
    NKB = S // 128
    GW = cfg.GRID_W
    RPC = TOK // GW
    KH = min(8, cfg.ROWS)
    NKBC = KH * GW // 128
    LT5 = TOK + S - 1
    LT5P = LT5 + 1

    nc = bass.Bass("TRN2", target_bir_lowering=False)
    P = Prog(nc)

    agc = {"n": 0}
    AG_BYTES = 512 * 1024

    def ag_nr(rows, cols):
        return max(1, min(rows, AG_BYTES // (cols * 2)))

    def allgather(src, dst, reads, writes, nr):
        rows, cols = src.shape
        agc["n"] += 1
        tag = agc["n"]
        g4 = [[0, 1, 2, 3], [4, 5, 6, 7]]
        g2 = [[0, 4], [1, 5], [2, 6], [3, 7]]
        if NCORES > 1:
            hb = [nc.dram_tensor("aghalf%d_%d" % (tag, i), [4 * nr, cols], BF16, kind="Internal").ap() for i in range(2)]
        b = 0
        k = 0
        while b < rows:
            n = min(nr, rows - b)
            d = dst[NCORES * b:NCORES * (b + n), :]
            sv = src[b:b + n, :]
            if NCORES == 1:
                P.dma("pool", lambda e, d=d, sv=sv: e.dma_start(out=d, in_=sv), "cc1", reads=reads, writes=writes)
            else:
                h = hb[k % 2][0:4 * n, :]
                hk = "hb%d_%d" % (tag, k % 2)
                P.dma("pool", lambda e, sv=sv, h=h: e.collective_compute("AllGather", ALU.bypass, replica_groups=g4, ins=[sv], outs=[h]),
                      "cc", reads=reads, writes=[hk], inc=1)
                P.dma("pool", lambda e, h=h, d=d: e.collective_compute("AllGather", ALU.bypass, replica_groups=g2, ins=[h], outs=[d]),
                      "cc", reads=[hk], writes=writes, inc=1)
            b += n
            k += 1

    def din(name, shape, dt=F32):
        return nc.dram_tensor(name, list(shape), dt, kind="ExternalInput").ap()

    def dint(name, shape, dt=BF16):
        return nc.dram_tensor(name, list(shape), dt, kind="Internal").ap()

    xT_in = din("xT", [D, TOK])
    wshards = {}
    wfull = {}
    wspec = [("w_in", DEPTH * D, IN_WIDTH), ("w_ba", DEPTH * 512, D), ("w_bb", DEPTH * 1024, D),
             ("w_bc", DEPTH * 512, D), ("w_out", DEPTH * D, D),
             ("f_gate", D, FFN), ("f_up", D, FFN), ("f_down", FFN, D),
             ("m_gate", NE * D, EDIM), ("m_up", NE * D, EDIM), ("m_down", NE * EDIM, D)]
    for nm, r, c in wspec:
        wshards[nm] = din(nm, [r // NCORES, c])
        wfull[nm] = dint("g_" + nm, [r, c])
    norm_mix = din("norm_mix", [DEPTH, D])
    norm_ffn = din("norm_ffn", [DEPTH, D])
    norm_final = din("norm_final", [1, D])
    diff_lambda = din("diff_lambda", [DEPTH, 256])
    diff_subln = din("diff_subln", [DEPTH, 128])
    qk_norm_b = din("qk_norm_b", [DEPTH, 256])
    moe_router = din("moe_router", [D, NE])
    cosT_d = din("cosT", [128, TOK])
    sinT_d = din("sinT", [128, TOK])
    rmat_d = din("rmat", [128, 128])
    ident_d = din("ident", [128, 128])
    t5vec_d = din("t5vec", [4, LT5P])
    idxC_d = din("idxC", [128, RPC * NKBC], I32)
    cbias_d = din("cbias", [DEPTH, 8, 128, 4 * NKBC * GW])
    out_d = nc.dram_tensor("outT", [D, TOK], F32, kind="ExternalOutput").ap()

    wcast = {nm: dint("c_" + nm, [r // NCORES, c]) for nm, r, c in wspec}
    xT_s = [dint("xT_s%d" % i, [D, TOK], F32) for i in range(2)]
    QT_s = dint("QT_s", [16, 128, TOK])
    KTc = dint("KTc", [6, 128, TOK])
    KT_all = dint("KT_all", [NCORES * 6, 128, TOK])
    Vc = dint("Vc", [TOK, 768])
    V_all = dint("V_all", [S, 768])
    Cc = dint("Cc", [TOK, 1024])
    C_all = dint("C_all", [S, 1024])
    yT_s = dint("yT_s", [16, 128, TOK])
    gT_s = dint("gT_s", [48, 128, TOK])
    t5skew = dint("t5skew", [4, 128, LT5P], F32)

    import contextlib
    es = contextlib.ExitStack()

    def sb(name, shape, dt=F32):
        return es.enter_context(nc.sbuf_tensor(name, list(shape), dt))

    with es:
        es.enter_context(nc.allow_non_contiguous_dma(reason="small constant loads / strided panels"))
        psum = es.enter_context(nc.psum_tensor("psum", [128, 8, 512], F32))
        ones_f = sb("ones_f", [128, 128])
        ident_f = sb("ident_f", [128, 128])
        ident_b = sb("ident_b", [128, 128], BF16)
        rmat_b = sb("rmat_b", [128, 128], BF16)
        epsD = sb("epsD", [128, 1])
        gmix = sb("gmix", [128, DEPTH, KC])
        gffn = sb("gffn", [128, DEPTH, KC])
        gfin = sb("gfin", [128, KC])
        qkg = sb("qkg", [128, DEPTH * 2])
        subln = sb("subln_bc", [128, DEPTH, 128])
        lamraw = sb("lamraw", [128, DEPTH, 256])
        lamv = sb("lamv", [128, DEPTH, 4])
        wr_f = sb("wr_f", [128, KC, NE])
        BIG = 158 * 1024
        big = sb("bigbuf", [128, BIG // 4], F32)

        def carve(off_bytes, shape, dt):
            esz = 4 if dt in (F32, I32) else 2
            n = int(np.prod(shape[1:]))
            assert off_bytes % 4 == 0
            assert off_bytes + n * esz <= BIG, (off_bytes, n * esz)
            if esz == 4:
                base = big[:, off_bytes // 4: off_bytes // 4 + n]
                if dt == I32:
                    base = base.bitcast(I32)
            else:
                base = big[:, off_bytes // 4: off_bytes // 4 + (n + 1) // 2].bitcast(BF16)[:, 0:n]
            if len(shape) == 2:
                return base
            names = " ".join("a%d" % i for i in range(len(shape) - 1))
            kw = {"a%d" % i: shape[i + 1] for i in range(len(shape) - 1)}
            return base.rearrange("p (%s) -> p %s" % (names, names), **kw)

        P.dma("sp", lambda e: e.dma_start(out=ident_f[:], in_=ident_d), "c0", writes=["ident_f"])
        P.dma("sp", lambda e: e.dma_start(out=gmix[:], in_=norm_mix.rearrange("l (k p) -> p l k", p=128)), "c0", writes=["gmix"])
        P.dma("sp", lambda e: e.dma_start(out=gffn[:], in_=norm_ffn.rearrange("l (k p) -> p l k", p=128)), "c0", writes=["gffn"])
        P.dma("sp", lambda e: e.dma_start(out=gfin[:], in_=norm_final.rearrange("o (k p) -> p (o k)", p=128)), "c0", writes=["gfin"])
        P.dma("sp", lambda e: e.dma_start(out=qkg[:], in_=qk_norm_b.rearrange("l (j p) -> p (l j)", p=128)), "c0", writes=["qkg"])
        P.dma("sp", lambda e: e.dma_start(out=subln[:], in_=diff_subln.rearrange("(o l) d -> o l d", o=1).broadcast_to([128, DEPTH, 128])), "c0", writes=["subln"])
        P.dma("sp", lambda e: e.dma_start(out=lamraw[:], in_=diff_lambda.rearrange("(o l) d -> o l d", o=1).broadcast_to([128, DEPTH, 256])), "c0", writes=["lamraw"])
        P.dma("sp", lambda e: e.dma_start(out=wr_f[:], in_=moe_router.rearrange("(k p) e -> p k e", p=128)), "c0", writes=["wr_f"])
        P.dma("pool", lambda e: e.dma_start(out=ident_b[:], in_=ident_d), "c1", writes=["ident_b"])
        P.dma("pool", lambda e: e.dma_start(out=rmat_b[:], in_=rmat_d), "c1", writes=["rmat_b"])
        P.op("dve", lambda e: e.memset(ones_f[:], 1.0), writes=["ones_f"])
        P.op("dve", lambda e: e.memset(epsD[:], EPS), writes=["epsD"])

        lamtmp = sb("lamtmp", [128, 64])
        lamacc = sb("lamacc", [128, DEPTH, 2])
        for l in range(DEPTH):
            for j in range(2):
                P.op("dve", lambda e, l=l, j=j: e.tensor_tensor(out=lamtmp[:], in0=lamraw[:, l, (2 * j) * 64:(2 * j + 1) * 64],
                                                              in1=lamraw[:, l, (2 * j + 1) * 64:(2 * j + 2) * 64], op=ALU.mult),
                     reads=["lamraw"], writes=["lamtmp"])
                P.op("dve", lambda e, l=l, j=j: e.reduce_sum(out=lamacc[:, l, j:j + 1], in_=lamtmp[:], axis=mybir.AxisListType.X),
                     reads=["lamtmp"], writes=["lamacc"])
            P.op("act", lambda e, l=l: e.activation(out=lamacc[:, l, :], in_=lamacc[:, l, :], func=AF.Exp), reads=["lamacc"], writes=["lamacc"])
            lam_init = 0.8 - 0.6 * math.exp(-0.3 * l)
            P.op("dve", lambda e, l=l: e.tensor_tensor(out=lamv[:, l, 0:1], in0=lamacc[:, l, 0:1], in1=lamacc[:, l, 1:2], op=ALU.subtract),
                 reads=["lamacc"], writes=["lamv"])
            P.op("dve", lambda e, l=l, li=lam_init: e.tensor_scalar(out=lamv[:, l, 1:2], in0=lamv[:, l, 0:1], scalar1=li, scalar2=-1.0, op0=ALU.add, op1=ALU.mult),
                 reads=["lamv"], writes=["lamv"])

        for h in range(4):
            for p in range(128):
                P.dma("sp", lambda e, h=h, p=p: e.dma_start(out=t5skew[h, p:p + 1, 0:LT5P - 127], in_=t5vec_d[h:h + 1, 127 - p:LT5P - p]),
                      "c2", writes=["t5skew"])

        def cast_and_gather(nm):
            src, dst = wshards[nm], wcast[nm]
            rows, cols = src.shape
            step = max(1, min(rows, 256, (1 << 20) // (cols * 4)))
            r = 0
            while r < rows:
                rr = min(step, rows - r)
                P.dma("pool", lambda e, r=r, rr=rr: e.dma_start(out=dst[r:r + rr, :], in_=src[r:r + rr, :]), "wc_" + nm, writes=["wc_" + nm])
                r += rr
            allgather(dst, wfull[nm], ["wc_" + nm], ["W_" + nm], ag_nr(rows, cols))

        for nm in ("w_in", "w_ba", "w_bb", "w_bc", "w_out", "f_gate", "f_up", "f_down"):
            cast_and_gather(nm)

        state = {"moe_gathered": False}

        psum_rr = {"i": 0}

        def load_panel(W, r0, kc_n, c0, ncols, slot_ap, slot_key, wkey, eng="sp", extra=()):
            src = W[r0:r0 + kc_n * 128, c0:c0 + ncols].rearrange("(k p) n -> p k n", p=128)
            kstep = max(1, 8192 // 128 // max(1, 1))
            kstep = min(kc_n, 32)
            k = 0
            while k < kc_n:
                kk = min(kstep, kc_n - k)
                P.dma(eng, lambda e, k=k, kk=kk: e.dma_start(out=slot_ap[:, k:k + kk, 0:ncols], in_=src[:, k:k + kk, :]),
                      "p_" + slot_key, reads=[wkey], writes=[slot_key] + list(extra))
                k += kk

        def rmsnorm_fm(xt, xkey, gain_ap_fn, tt_tokens, hT_out, hkey, sq, sqkey, rstd, rkey, psb, pskey, h32=None, h32key=None):
            ntok = tt_tokens
            for kc in range(KC):
                P.op("act", lambda e, kc=kc: e.activation(out=sq[:, 0:ntok], in_=xt[:, kc, :], func=AF.Square), reads=[xkey], writes=[sqkey])
                P.op("pe", lambda e, kc=kc: e.matmul(psb[:, 0:ntok], lhsT=ones_f[:], rhs=sq[:, 0:ntok], start=(kc == 0), stop=(kc == KC - 1)),
                     reads=[sqkey, "ones_f"], writes=[pskey])
            P.op("act", lambda e: e.activation(out=rstd[:, 0:ntok], in_=psb[:, 0:ntok], func=AF.Sqrt, scale=1.0 / D, bias=epsD[:, 0:1]),
                 reads=[pskey, "epsD"], writes=[rkey])
            P.op("dve", lambda e: e.reciprocal(out=rstd[:, 0:ntok], in_=rstd[:, 0:ntok]), reads=[rkey], writes=[rkey])
            for kc in range(KC):
                P.op("dve", lambda e, kc=kc: e.scalar_tensor_tensor(out=hT_out[:, kc, :], in0=xt[:, kc, :], scalar=gain_ap_fn(kc), in1=rstd[:, 0:ntok],
                                                                   op0=ALU.mult, op1=ALU.mult),
                     reads=[xkey, rkey], writes=[hkey])
                if h32 is not None:
                    P.op("dve", lambda e, kc=kc: e.scalar_tensor_tensor(out=h32[:, kc, :], in0=xt[:, kc, :], scalar=gain_ap_fn(kc), in1=rstd[:, 0:ntok],
                                                                       op0=ALU.mult, op1=ALU.mult),
                         reads=[xkey, rkey], writes=[h32key])

        def layer(l, x_src, x_dst):
            w_in_l = wfull["w_in"][l * D:(l + 1) * D, :]
            hT = carve(0, [128, KC, TOK], BF16)
            o1 = KC * TOK * 2
            xt = [carve(o1 + i * KC * 512 * 4, [128, KC, 512], F32) for i in range(1)]
            o2 = o1 + KC * 512 * 4
            sq = carve(o2, [128, 512], F32)
            rstd = carve(o2 + 2048, [128, 512], F32)
            o3 = o1
            for tt in range(NTT):
                P.dma("sp", lambda e, tt=tt: e.dma_start(out=xt[0], in_=x_src[:, tt * 512:(tt + 1) * 512].rearrange("(k p) t -> p k t", p=128)),
                      "xt", reads=["X%d" % l], writes=["xt"])
                rmsnorm_fm(xt[0], "xt", lambda kc, l=l: gmix[:, l, kc:kc + 1], 512, hT[:, :, tt * 512:(tt + 1) * 512], "hT",
                           sq, "sq", rstd, "rstd", psum[:, 7, :], "ps7")
            P.barrier()
            wbuf = [carve(o3 + i * KC * 512 * 2, [128, KC, 512], BF16) for i in range(2)]
            o4 = o3 + 2 * KC * 512 * 2
            stage = [carve(o4 + i * TOK * 2, [128, TOK], BF16) for i in range(2)]
            o5 = o4 + 2 * TOK * 2
            qn_b = carve(o5, [128, 512], BF16)
            t1 = carve(o5 + 1024, [128, 512], F32)
            t2 = carve(o5 + 1024 + 2048, [128, 512], F32)
            sq2 = carve(o5 + 1024 + 4096, [128, 512], F32)
            rt2 = carve(o5 + 1024 + 6144, [128, 512], F32)
            o6 = o5 + 1024 + 8192
            vst = [carve(o6 + i * 1024, [128, 512], BF16) for i in range(2)]
            o7 = o6 + 2048
            cosT = carve(o7, [128, TOK], F32)
            sinT = carve(o7 + TOK * 4, [128, TOK], F32)
            P.dma("sp", lambda e, cosT=cosT: e.dma_start(out=cosT, in_=cosT_d), "c0", writes=["cosT"])
            P.dma("sp", lambda e, sinT=sinT: e.dma_start(out=sinT, in_=sinT_d), "c0", writes=["sinT"])

            npanel = IN_WIDTH // 512
            stage_i = {"i": 0}
            vst_i = {"i": 0}
            for pn in range(npanel):
                slot = pn % 2
                c0 = pn * 512
                load_panel(w_in_l, 0, KC, c0, 512, wbuf[slot], "wbuf%d" % slot, "W_w_in")
                def kind_of(col):
                    if col < 512: return ("qa", col // 128)
                    if col < 1024: return ("ka", (col - 512) // 128)
                    if col < 1536: return ("va", 0)
                    if col < 2560: return ("qb", (col - 1536) // 128)
                    if col < 2816: return ("kb", (col - 2560) // 128)
                    if col < 3072: return ("vb", 0)
                    if col < 3584: return ("qc", (col - 3072) // 128)
                    if col < 4096: return ("kc", 0)
                    if col < 4608: return ("vc", 0)
                    return ("gz", (col - 4608) // 128)
                cc = 0
                while cc < 512:
                    col = c0 + cc
                    kind, hidx = kind_of(col)
                    if kind in ("va", "vb", "kc", "vc"):
                        gend = {"va": 1536, "vb": 3072, "kc": 4096, "vc": 4608}[kind]
                        ncol = min(gend, c0 + 512) - col
                        gstart = {"va": 1024, "vb": 2816, "kc": 3584, "vc": 4096}[kind]
                        for tb in range(TOK // 128):
                            bank = psum_rr["i"] % 4
                            psum_rr["i"] += 1
                            for kc in range(KC):
                                P.op("pe", lambda e, kc=kc, tb=tb, bank=bank, cc=cc, ncol=ncol, slot=slot:
                                     e.matmul(psum[:, bank, 0:ncol], lhsT=hT[:, kc, tb * 128:(tb + 1) * 128], rhs=wbuf[slot][:, kc, cc:cc + ncol],
                                              start=(kc == 0), stop=(kc == KC - 1)),
                                     reads=["hT", "wbuf%d" % slot], writes=["ps%d" % bank])
                            vs = vst_i["i"] % 2
                            vst_i["i"] += 1
                            P.op("act", lambda e, bank=bank, ncol=ncol, vs=vs: e.activation(out=vst[vs][:, 0:ncol], in_=psum[:, bank, 0:ncol], func=AF.Copy),
                                 reads=["ps%d" % bank], writes=["vst%d" % vs])
                            if kind == "va":
                                dst = Vc[tb * 128:(tb + 1) * 128, col - gstart:col - gstart + ncol]
                            elif kind == "vb":
                                dst = Vc[tb * 128:(tb + 1) * 128, 512 + col - gstart:512 + col - gstart + ncol]
                            elif kind == "kc":
                                dst = Cc[tb * 128:(tb + 1) * 128, col - gstart:col - gstart + ncol]
                            else:
                                dst = Cc[tb * 128:(tb + 1) * 128, 512 + col - gstart:512 + col - gstart + ncol]
                            P.dma("sp", lambda e, dst=dst, vs=vs, ncol=ncol: e.dma_start(out=dst, in_=vst[vs][:, 0:ncol]), "vst%d" % vs,
                                  reads=["vst%d" % vs], writes=["KV%d" % l])
                        cc += ncol
                        continue
                    st = stage_i["i"] % 2
                    stage_i["i"] += 1
                    for tt in range(NTT):
                        bank = psum_rr["i"] % 4
                        psum_rr["i"] += 1
                        for kc in range(KC):
                            P.op("pe", lambda e, kc=kc, tt=tt, bank=bank, cc=cc, slot=slot:
                                 e.matmul(psum[:, bank, :], lhsT=wbuf[slot][:, kc, cc:cc + 128], rhs=hT[:, kc, tt * 512:(tt + 1) * 512],
                                          start=(kc == 0), stop=(kc == KC - 1)),
                                 reads=["hT", "wbuf%d" % slot], writes=["ps%d" % bank])
                        dsts = stage[st][:, tt * 512:(tt + 1) * 512]
                        skey = "stage%d" % st
                        if kind in ("qa", "ka", "qc"):
                            P.op("act", lambda e, bank=bank, dsts=dsts: e.activation(out=dsts, in_=psum[:, bank, :], func=AF.Copy),
                                 reads=["ps%d" % bank], writes=[skey])
                        elif kind == "gz":
                            P.op("act", lambda e, bank=bank, dsts=dsts: e.activation(out=dsts, in_=psum[:, bank, :], func=AF.Sigmoid),
                                 reads=["ps%d" % bank], writes=[skey])
                        else:
                            gcol = l * 2 + (0 if kind == "qb" else 1)
                            P.op("act", lambda e, bank=bank: e.activation(out=sq2[:], in_=psum[:, bank, :], func=AF.Square), reads=["ps%d" % bank], writes=["sq2"])
                            P.op("pe", lambda e: e.matmul(psum[:, 4, :], lhsT=ones_f[:], rhs=sq2[:], start=True, stop=True), reads=["sq2", "ones_f"], writes=["ps4"])
                            P.op("act", lambda e: e.activation(out=rt2[:], in_=psum[:, 4, :], func=AF.Sqrt, scale=1.0 / 128, bias=epsD[:, 0:1]), reads=["ps4"], writes=["rt2"])
                            P.op("dve", lambda e: e.reciprocal(out=rt2[:], in_=rt2[:]), reads=["rt2"], writes=["rt2"])
                            P.op("dve", lambda e, bank=bank, gcol=gcol: e.scalar_tensor_tensor(out=qn_b[:], in0=psum[:, bank, :], scalar=qkg[:, gcol:gcol + 1], in1=rt2[:],
                                                                                         op0=ALU.mult, op1=ALU.mult),
                                 reads=["ps%d" % bank, "rt2", "qkg"], writes=["qn_b"])
                            P.op("pe", lambda e: e.matmul(psum[:, 5, :], lhsT=rmat_b[:], rhs=qn_b[:], start=True, stop=True), reads=["qn_b", "rmat_b"], writes=["ps5"])
                            P.op("dve", lambda e, tt=tt: e.tensor_tensor(out=t1[:], in0=qn_b[:], in1=cosT[:, tt * 512:(tt + 1) * 512], op=ALU.mult),
                                 reads=["qn_b", "cosT"], writes=["t1"])
                            P.op("dve", lambda e, tt=tt: e.tensor_tensor(out=t2[:], in0=psum[:, 5, :], in1=sinT[:, tt * 512:(tt + 1) * 512], op=ALU.mult),
                                 reads=["ps5", "sinT"], writes=["t2"])
                            P.op("pool", lambda e, dsts=dsts: e.tensor_tensor(out=dsts, in0=t1[:], in1=t2[:], op=ALU.add), reads=["t1", "t2"], writes=[skey])
                    if kind == "qa":
                        dst, wk = QT_s[hidx], "QT%d" % l
                    elif kind == "qb":
                        dst, wk = QT_s[4 + hidx], "QT%d" % l
                    elif kind == "qc":
                        dst, wk = QT_s[12 + hidx], "QT%d" % l
                    elif kind == "ka":
                        dst, wk = KTc[hidx], "KV%d" % l
                    elif kind == "kb":
                        dst, wk = KTc[4 + hidx], "KV%d" % l
                    else:
                        dst, wk = gT_s[hidx], "GT%d" % l
                    P.dma("sp", lambda e, dst=dst, st=st: e.dma_start(out=dst, in_=stage[st][:]), "stage%d" % st, reads=["stage%d" % st], writes=[wk])
                    cc += 128

            allgather(KTc.rearrange("h p t -> (h p) t"), KT_all.rearrange("h p t -> (h p) t"), ["KV%d" % l], ["KTall%d" % l], 128)
            allgather(Vc, V_all, ["KV%d" % l], ["Vall%d" % l], min(256, TOK))
            allgather(Cc, C_all, ["KV%d" % l], ["Call%d" % l], min(256, TOK))
            if not state["moe_gathered"]:
                for nm in ("m_gate", "m_up", "m_down"):
                    cast_and_gather(nm)
                state["moe_gathered"] = True

            if debug_stop == "inproj":
                return True
            P.barrier()

            KTb = carve(0, [128, S], BF16)
            a1 = S * 2
            Vb = carve(a1, [128, NKB, 130], BF16)
            a2 = a1 + NKB * 130 * 2
            a2 = (a2 + 3) // 4 * 4
            Qh = carve(a2, [128, TOK], BF16)
            a3 = a2 + TOK * 2
            Pb = [carve(a3 + i * 1024, [128, 512], BF16) for i in range(4)]
            a4 = a3 + 4096
            tb_ = [carve(a4 + i * 2048, [128, 512], F32) for i in range(2)]
            a5 = a4 + 4096
            bt = [carve(a5 + i * 2048, [128, 512], F32) for i in range(2)]
            a6 = a5 + 4096
            of = carve(a6, [128, 4, 130], F32)
            a7 = a6 + 4 * 130 * 4
            of2 = carve(a7, [128, 4, 128], F32)
            a8 = a7 + 2048
            rinv = carve(a8, [128, 16], F32)
            a9 = a8 + 64
            ob = carve(a9, [128, 4, 128], BF16)
            a10 = a9 + 1024
            yst = carve(a10, [128, TOK], BF16)
            a11 = a10 + TOK * 2

            def load_V(col0, hkey_reads):
                nrv = min(256, TOK)
                J = nrv // 128
                v5 = V_all.rearrange("(k r j p) d -> r p k j d", r=NCORES, j=J, p=128)
                bpr = TOK // 128
                for r in range(NCORES):
                    for k in range(bpr // J):
                        kb0 = r * bpr + k * J
                        P.dma("sp", lambda e, r=r, k=k, kb0=kb0: e.dma_start(out=Vb[:, kb0:kb0 + J, 0:128], in_=v5[r][:, k, :, col0:col0 + 128]), "Vb",
                              reads=hkey_reads, writes=["Vb"])
                P.op("pool", lambda e: e.memset(Vb[:, :, 128:129], 1.0), writes=["Vb"])

            def load_KT(hh, reads):
                src = KT_all.rearrange("(h r) p t -> h p r t", r=NCORES)[hh]
                for r in range(NCORES):
                    P.dma("sp", lambda e, r=r: e.dma_start(out=KTb[:, r * TOK:(r + 1) * TOK], in_=src[:, r, :]), "KTb", reads=reads, writes=["KTb"])

            def pv_accum(pbuf, pkey, kb, banks, first):
                for s in range(4):
                    bank = banks[s // 2]
                    cofs = (s % 2) * 130
                    P.op("pe", lambda e, s=s, bank=bank, cofs=cofs, kb=kb, pbuf=pbuf, first=first:
                         e.matmul(psum[:, bank, cofs:cofs + 129], lhsT=pbuf[:, s * 128:(s + 1) * 128], rhs=Vb[:, kb, 0:129],
                                  start=(first and s % 2 == 0), stop=False, skip_group_check=True),
                         reads=[pkey, "Vb"], writes=["ps%d" % bank])

            pb_i = {"i": 0}
            for g in range(2):
                load_KT(4 + g, ["KTall%d" % l])
                load_V(512 + g * 128, ["Vall%d" % l])
                for hq in range(4):
                    h = g * 4 + hq
                    P.dma("sp", lambda e, h=h: e.dma_start(out=Qh[:], in_=QT_s[4 + h]), "Qh", reads=["QT%d" % l], writes=["Qh"])
                    for qt in range(NTT):
                        for kb in range(NKB):
                            sbank = kb % 2
                            P.op("pe", lambda e, kb=kb, qt=qt, sbank=sbank: e.matmul(psum[:, sbank, :], lhsT=KTb[:, kb * 128:(kb + 1) * 128],
                                                                                   rhs=Qh[:, qt * 512:(qt + 1) * 512], start=True, stop=True),
                                 reads=["KTb", "Qh"], writes=["ps%d" % sbank])
                            pi = pb_i["i"] % 4
                            pb_i["i"] += 1
                            P.op("act", lambda e, sbank=sbank, pi=pi: e.activation(out=Pb[pi][:], in_=psum[:, sbank, :], func=AF.Exp, scale=128 ** -0.5),
                                 reads=["ps%d" % sbank], writes=["Pb%d" % pi])
                            pv_accum(Pb[pi], "Pb%d" % pi, kb, (2, 3), kb == 0)
                        for bk in range(2):
                            P.op("dve", lambda e, bk=bk: e.tensor_copy(out=of[:, bk * 2:(bk + 1) * 2, :], in_=psum[:, 2 + bk, 0:260].rearrange("p (s c) -> p s c", c=130)),
                                 reads=["ps%d" % (2 + bk)], writes=["of"])
                        for s in range(4):
                            P.op("dve", lambda e, s=s: e.reciprocal(out=rinv[:, s:s + 1], in_=of[:, s, 128:129]), reads=["of"], writes=["rinv"])
                            P.op("dve", lambda e, s=s: e.tensor_scalar(out=ob[:, s, :], in0=of[:, s, 0:128], scalar1=rinv[:, s:s + 1], scalar2=None, op0=ALU.mult),
                                 reads=["of", "rinv"], writes=["ob"])
                        pb16 = psum[:, 4, 0:256].bitcast(BF16)
                        for s in range(4):
                            P.op("pe", lambda e, s=s, pb16=pb16: e.transpose(pb16[:, s * 128:(s + 1) * 128], ob[:, s, :], ident_b[:]),
                                 reads=["ob", "ident_b"], writes=["ps4"])
                        P.op("act", lambda e, qt=qt, pb16=pb16: e.activation(out=yst[:, qt * 512:(qt + 1) * 512], in_=pb16, func=AF.Copy), reads=["ps4"], writes=["yst"])
                    P.dma("sp", lambda e, h=h: e.dma_start(out=yT_s[4 + h], in_=yst[:]), "yst", reads=["yst"], writes=["YT%d" % l])

            if debug_stop == "attnB":
                return True

            subs = carve(a11, [128, 128], F32)
            a12 = a11 + 512
            ofb = carve(a12, [128, 4, 130], F32)
            a13 = a12 + 4 * 130 * 4
            ss = carve(a13, [128, 16], F32)
            a14 = a13 + 64
            sqt = carve(a14, [128, 128], F32)
            a15 = a14 + 512
            lam_init = 0.8 - 0.6 * math.exp(-0.3 * l)
            P.op("dve", lambda e, li=lam_init: e.tensor_scalar(out=subs, in0=subln[:, l, :], scalar1=1.0 - li, scalar2=None, op0=ALU.mult),
                 reads=["subln"], writes=["subs"])
            bt_i = {"i": 0}
            tb_i = {"i": 0}
            for h in range(4):
                load_KT(h, ["KTall%d" % l])
                load_V(h * 128, ["Vall%d" % l])
                P.dma("sp", lambda e, h=h: e.dma_start(out=Qh[:], in_=QT_s[h]), "Qh", reads=["QT%d" % l], writes=["Qh"])
                for qt in range(NTT):
                    for kb in range(NKB):
                        bi = bt_i["i"] % 2
                        bt_i["i"] += 1
                        off = qt * 512 - kb * 128 + S - 128
                        P.dma("sp", lambda e, bi=bi, off=off, h=h: e.dma_start(out=bt[bi], in_=t5skew[h, :, off:off + 512]), "bt%d" % bi,
                              reads=["t5skew"], writes=["bt%d" % bi])
                        for mp in range(2):
                            P.op("pe", lambda e, kb=kb, qt=qt, mp=mp: e.matmul(psum[:, mp, :], lhsT=KTb[mp * 64:(mp + 1) * 64, kb * 128:(kb + 1) * 128],
                                                                           rhs=Qh[mp * 64:(mp + 1) * 64, qt * 512:(qt + 1) * 512], start=True, stop=True),
                                 reads=["KTb", "Qh"], writes=["ps%d" % mp])
                        for mp in range(2):
                            ti = tb_i["i"] % 2
                            tb_i["i"] += 1
                            P.op("dve", lambda e, mp=mp, ti=ti, bi=bi: e.scalar_tensor_tensor(out=tb_[ti], in0=psum[:, mp, :], scalar=64 ** -0.5, in1=bt[bi],
                                                                                        op0=ALU.mult, op1=ALU.add),
                                 reads=["ps%d" % mp, "bt%d" % bi], writes=["tb%d" % ti])
                            pi = pb_i["i"] % 4
                            pb_i["i"] += 1
                            P.op("act", lambda e, ti=ti, pi=pi: e.activation(out=Pb[pi][:], in_=tb_[ti], func=AF.Exp), reads=["tb%d" % ti], writes=["Pb%d" % pi])
                            pv_accum(Pb[pi], "Pb%d" % pi, kb, (2, 3) if mp == 0 else (4, 5), kb == 0)
                    for bk in range(2):
                        P.op("dve", lambda e, bk=bk: e.tensor_copy(out=of[:, bk * 2:(bk + 1) * 2, :], in_=psum[:, 2 + bk, 0:260].rearrange("p (s c) -> p s c", c=130)),
                             reads=["ps%d" % (2 + bk)], writes=["of"])
                        P.op("dve", lambda e, bk=bk: e.tensor_copy(out=ofb[:, bk * 2:(bk + 1) * 2, :], in_=psum[:, 4 + bk, 0:260].rearrange("p (s c) -> p s c", c=130)),
                             reads=["ps%d" % (4 + bk)], writes=["ofb"])
                    for s_ in range(4):
                        P.op("dve", lambda e, s_=s_: e.reciprocal(out=rinv[:, s_:s_ + 1], in_=of[:, s_, 128:129]), reads=["of"], writes=["rinv"])
                        P.op("dve", lambda e, s_=s_: e.reciprocal(out=rinv[:, 4 + s_:5 + s_], in_=ofb[:, s_, 128:129]), reads=["ofb"], writes=["rinv"])
                        P.op("dve", lambda e, s_=s_: e.tensor_scalar(out=rinv[:, 4 + s_:5 + s_], in0=rinv[:, 4 + s_:5 + s_], scalar1=lamv[:, l, 1:2], scalar2=None, op0=ALU.mult),
                             reads=["rinv", "lamv"], writes=["rinv"])
                        P.op("dve", lambda e, s_=s_: e.tensor_scalar(out=of2[:, s_, :], in0=of[:, s_, 0:128], scalar1=rinv[:, s_:s_ + 1], scalar2=None, op0=ALU.mult),
                             reads=["of", "rinv"], writes=["of2"])
                        P.op("dve", lambda e, s_=s_: e.scalar_tensor_tensor(out=of2[:, s_, :], in0=ofb[:, s_, 0:128], scalar=rinv[:, 4 + s_:5 + s_], in1=of2[:, s_, :],
                                                                           op0=ALU.mult, op1=ALU.add),
                             reads=["ofb", "rinv", "of2"], writes=["of2"])
                        P.op("dve", lambda e, s_=s_: e.tensor_tensor(out=sqt, in0=of2[:, s_, :], in1=of2[:, s_, :], op=ALU.mult), reads=["of2"], writes=["sqt"])
                        P.op("dve", lambda e, s_=s_: e.reduce_sum(out=ss[:, s_:s_ + 1], in_=sqt, axis=mybir.AxisListType.X), reads=["sqt"], writes=["ss"])
                        P.op("act", lambda e, s_=s_: e.activation(out=ss[:, s_:s_ + 1], in_=ss[:, s_:s_ + 1], func=AF.Sqrt, scale=1.0 / 128, bias=epsD[:, 0:1]),
                             reads=["ss", "epsD"], writes=["ss"])
                        P.op("dve", lambda e, s_=s_: e.reciprocal(out=ss[:, s_:s_ + 1], in_=ss[:, s_:s_ + 1]), reads=["ss"], writes=["ss"])
                        P.op("dve", lambda e, s_=s_: e.scalar_tensor_tensor(out=ob[:, s_, :], in0=of2[:, s_, :], scalar=ss[:, s_:s_ + 1], in1=subs,
                                                                           op0=ALU.mult, op1=ALU.mult),
                             reads=["of2", "ss", "subs"], writes=["ob"])
                    pb16 = psum[:, 6, 0:256].bitcast(BF16)
                    for s_ in range(4):
                        P.op("pe", lambda e, s_=s_, pb16=pb16: e.transpose(pb16[:, s_ * 128:(s_ + 1) * 128], ob[:, s_, :], ident_b[:]),
                             reads=["ob", "ident_b"], writes=["ps6"])
                    P.op("act", lambda e, qt=qt, pb16=pb16: e.activation(out=yst[:, qt * 512:(qt + 1) * 512], in_=pb16, func=AF.Copy), reads=["ps6"], writes=["yst"])
                P.dma("sp", lambda e, h=h: e.dma_start(out=yT_s[h], in_=yst[:]), "yst", reads=["yst"], writes=["YT%d" % l])

            if debug_stop == "attnA":
                return True
            P.barrier()
            NQ = GW
            Qc = carve(0, [128, 4, TOK], BF16)
            c1 = 4 * TOK * 2
            cb_sb = carve(c1, [128, 8, 4 * NKBC * GW], F32)
            c2 = c1 + 8 * 4 * NKBC * GW * 4
            Cg = [carve(c2 + i * NKBC * 1024 * 2, [128, NKBC, 1024], BF16) for i in range(2)]
            c3 = c2 + 2 * NKBC * 1024 * 2
            KcT = carve(c3, [128, 4, NKBC * 128], BF16)
            c4 = c3 + 4 * NKBC * 128 * 2
            Vcg = carve(c4, [128, NKBC, 4, 130], BF16)
            c5 = c4 + NKBC * 4 * 130 * 2
            tC = carve(c5, [128, NKBC * GW], F32)
            c6 = c5 + NKBC * GW * 4
            PC = carve(c6, [128, NKBC, GW], BF16)
            c7 = c6 + NKBC * GW * 2
            obc = carve(c7, [128, 128], BF16)
            c8 = c7 + 256
            rc = carve(c8, [128, 4], F32)
            c9 = c8 + 16
            ycst = carve(c9, [128, 4, TOK], BF16)
            c10 = c9 + 4 * TOK * 2
            idx_sb = carve(c10, [128, RPC * NKBC], I32)
            P.dma("sp", lambda e: e.dma_start(out=idx_sb, in_=idxC_d), "c3", writes=["idx_sb"])
            P.dma("sp", lambda e: e.dma_start(out=cb_sb, in_=cbias_d[l].rearrange("c p f -> p c f")), "c3", writes=["cb_sb"])
            for h in range(4):
                P.dma("sp", lambda e, h=h: e.dma_start(out=Qc[:, h, :], in_=QT_s[12 + h]), "c3", reads=["QT%d" % l], writes=["Qc"])
            P.op("pool", lambda e: e.memset(Vcg[:, :, :, 128:129], 1.0), writes=["Vcg"])
            for rl in range(RPC):
                gi = rl % 2
                cls = rl if rl < 4 else (4 if rl < RPC - 3 else 8 - (RPC - rl))
                for kb in range(NKBC):
                    P.dma("pool", lambda e, gi=gi, kb=kb, rl=rl: e.indirect_dma_start(
                        out=Cg[gi][:, kb, :], out_offset=None, in_=C_all,
                        in_offset=bass.IndirectOffsetOnAxis(ap=idx_sb[:, rl * NKBC + kb:rl * NKBC + kb + 1], axis=0)),
                        "Cg%d" % gi, reads=["Call%d" % l, "idx_sb", "W_m_gate", "W_m_up", "W_m_down"], writes=["Cg%d" % gi])
                import os
                CM = int(os.environ.get("CMODE", "9"))
                if CM < 2:
                    continue
                for kb in range(NKBC):
                    pbk = psum[:, kb % 2, 0:256].bitcast(BF16)
                    for h in range(4):
                        P.op("pe", lambda e, gi=gi, kb=kb, h=h, pbk=pbk: e.transpose(pbk[:, h * 128:(h + 1) * 128], Cg[gi][:, kb, h * 128:(h + 1) * 128], ident_b[:]),
                             reads=["Cg%d" % gi, "ident_b"], writes=["ps%d" % (kb % 2)])
                    P.op("act", lambda e, kb=kb, pbk=pbk: e.activation(out=KcT[:, :, kb * 128:(kb + 1) * 128], in_=pbk.rearrange("p (h k) -> p h k", h=4), func=AF.Copy),
                         reads=["ps%d" % (kb % 2)], writes=["KcT"])
                    P.op("pool", lambda e, gi=gi, kb=kb: e.tensor_copy(out=Vcg[:, kb, :, 0:128], in_=Cg[gi][:, kb, 512:1024].rearrange("p (h d) -> p h d", h=4)),
                         reads=["Cg%d" % gi], writes=["Vcg"])
                for h in range(4):
                    if CM < 3:
                        continue
                    sb_ = 2 + (h % 2)
                    for kb in range(NKBC):
                        P.op("pe", lambda e, h=h, kb=kb, rl=rl, sb_=sb_: e.matmul(psum[:, sb_, kb * GW:(kb + 1) * GW], lhsT=KcT[:, h, kb * 128:(kb + 1) * 128],
                                                                               rhs=Qc[:, h, rl * GW:(rl + 1) * GW], start=True, stop=True),
                             reads=["KcT", "Qc"], writes=["ps%d" % sb_])
                    P.op("dve", lambda e, h=h, cls=cls, sb_=sb_: e.scalar_tensor_tensor(out=tC, in0=psum[:, sb_, 0:NKBC * GW], scalar=128 ** -0.5,
                                                                                 in1=cb_sb[:, cls, h * NKBC * GW:(h + 1) * NKBC * GW], op0=ALU.mult, op1=ALU.add),
                         reads=["ps%d" % sb_, "cb_sb"], writes=["tC"])
                    P.op("act", lambda e: e.activation(out=PC.rearrange("p k q -> p (k q)"), in_=tC, func=AF.Exp), reads=["tC"], writes=["PC"])
                    if CM < 4:
                        continue
                    ob_ = 4 + (h % 2)
                    for kb in range(NKBC):
                        P.op("pe", lambda e, h=h, kb=kb, ob_=ob_: e.matmul(psum[0:GW, ob_, 0:129], lhsT=PC[:, kb, :], rhs=Vcg[:, kb, h, 0:129],
                                                                         start=(kb == 0), stop=(kb == NKBC - 1)),
                             reads=["PC", "Vcg"], writes=["ps%d" % ob_])
                    P.op("dve", lambda e, ob_=ob_: e.reciprocal(out=rc[0:GW, 0:1], in_=psum[0:GW, ob_, 128:129]), reads=["ps%d" % ob_], writes=["rc"])
                    P.op("dve", lambda e, ob_=ob_: e.tensor_scalar(out=obc[0:GW, :], in0=psum[0:GW, ob_, 0:128], scalar1=rc[0:GW, 0:1], scalar2=None, op0=ALU.mult),
                         reads=["ps%d" % ob_, "rc"], writes=["obc"])
                    if CM < 5:
                        continue
                    pt = psum[:, 6 + (h % 2), 0:32].bitcast(BF16)
                    P.op("pe", lambda e, pt=pt: e.transpose(pt[:, 0:GW], obc[0:GW, :], ident_b[0:GW, 0:GW]), reads=["obc", "ident_b"], writes=["ps%d" % (6 + h % 2)])
                    P.op("act", lambda e, h=h, rl=rl, pt=pt: e.activation(out=ycst[:, h, rl * GW:(rl + 1) * GW], in_=pt[:, 0:GW], func=AF.Copy),
                         reads=["ps%d" % (6 + h % 2)], writes=["ycst"])
            for h in range(4):
                P.dma("sp", lambda e, h=h: e.dma_start(out=yT_s[12 + h], in_=ycst[:, h, :]), "c3", reads=["ycst"], writes=["YT%d" % l])
            if debug_stop == "attnC":
                return True
            P.barrier()

            is_moe = (l % 2 == 1)
            nff = (EDIM if is_moe else FFN) // 128
            FG = max(g_ for g_ in range(1, 15) if nff % g_ == 0)
            xt3 = carve(0, [128, KC, 512], F32)
            p1 = KC * 512 * 4
            ytt = carve(p1, [128, KC, 512], BF16)
            mrg = carve(p1 + KC * 512 * 2, [128, KC, 512], BF16)
            h32 = carve(p1, [128, KC, 512], F32)
            p2 = p1 + KC * 512 * 4
            h2T = carve(p2, [128, KC, 512], BF16)
            p3 = p2 + KC * 512 * 2
            aT = carve(p3, [128, FG, 512], BF16)
            p4 = p3 + 14 * 512 * 2
            wsl = [carve(p4 + i * KC * 512 * 2, [128, KC, 512], BF16) for i in range(2)]
            wgu = [carve(p4 + i * KC * 256 * 2, [128, KC, 256], BF16) for i in range(4)]
            if os.environ.get("WGU"):
                wgu = [wsl[0][:, :, 0:256], wsl[0][:, :, 256:512], wsl[1][:, :, 0:256], wsl[1][:, :, 256:512]]
            p5 = p4 + 2 * KC * 512 * 2
            wdn = [carve(p5 + i * 14 * 128 * 2, [128, FG, 128], BF16) for i in range(2)]
            p6 = p5 + 2 * 14 * 128 * 2
            gtl = [carve(p6 + i * 3 * 512 * 2, [128, 3, 512], BF16) for i in range(2)]
            p7 = p6 + 2 * 3 * 512 * 2
            tm = [carve(p7 + i * 2048, [128, 512], F32) for i in range(3)]
            p8 = p7 + 3 * 2048
            sg = [carve(p8 + i * 2048, [128, 512], F32) for i in range(2)]
            p9 = p8 + 2 * 2048
            sq3 = carve(p9, [128, 512], F32)
            rstd3 = carve(p9 + 2048, [128, 512], F32)
            p10 = p9 + 4096
            wrep = carve(p10, [128, 512], F32)
            p11 = p10 + 2048
            lg = carve(p11, [128, 8], F32)
            m8 = carve(p11 + 32, [128, 8], F32)
            gg = carve(p11 + 64, [128, 8], F32)
            wmat = carve(p11 + 96, [128, 4, 8], F32)
            w2 = carve(p11 + 224, [128, 8], F32)
            p12 = p11 + 256
            wbc = carve(p12, [128, 128], F32)
            p13 = p12 + 512
            outf = carve(p1, [128, KC, 512], F32)

            wba = wfull["w_ba"][l * 512:(l + 1) * 512, :]
            wbb = wfull["w_bb"][l * 1024:(l + 1) * 1024, :]
            wbc_w = wfull["w_bc"][l * 512:(l + 1) * 512, :]
            wout = wfull["w_out"][l * D:(l + 1) * D, :]
            pr = {"i": 0}
            gu_i = {"i": 0}

            def nextbank():
                b = pr["i"] % 6
                pr["i"] += 1
                return b

            for tt in range(NTT):
                tsl = slice(tt * 512, (tt + 1) * 512)
                P.dma("sp", lambda e, tsl=tsl: e.dma_start(out=xt3, in_=x_src[:, tsl].rearrange("(k p) t -> p k t", p=128)), "xt3", reads=["X%d" % l], writes=["xt3"])
                P.dma("sp", lambda e, tsl=tsl: e.dma_start(out=ytt, in_=yT_s[:, :, tsl].rearrange("c p t -> p c t")), "ytt", reads=["YT%d" % l], writes=["ytt"])
                import os
                PM = int(os.environ.get("PMODE", "9"))
                for pn in range(D // 512 if PM >= 2 else 0):
                    slot = pn % 2
                    ex_ = ["wgu%d" % (2 * slot), "wgu%d" % (2 * slot + 1)]
                    load_panel(wba, 0, 4, pn * 512, 512, wsl[slot][:, 0:4, :], "wsl%d" % slot, "W_w_ba", extra=ex_)
                    load_panel(wbb, 0, 8, pn * 512, 512, wsl[slot][:, 4:12, :], "wsl%d" % slot, "W_w_bb", extra=ex_)
                    load_panel(wbc_w, 0, 4, pn * 512, 512, wsl[slot][:, 12:16, :], "wsl%d" % slot, "W_w_bc", extra=ex_)
                    for j in range(4):
                        nch = pn * 4 + j
                        gi = nch % 2
                        P.dma("sp", lambda e, gi=gi, nch=nch, tsl=tsl: e.dma_start(
                            out=gtl[gi], in_=gT_s.rearrange("(b n) p t -> n p b t", b=3)[nch][:, :, tsl]), "gtl%d" % gi, reads=["GT%d" % l], writes=["gtl%d" % gi])
                        banks = []
                        for b, (k0, k1) in enumerate(((0, 4), (4, 12), (12, 16))):
                            bank = nextbank()
                            banks.append(bank)
                            for kc in range(k0, k1):
                                P.op("pe", lambda e, kc=kc, bank=bank, slot=slot, j=j, k0=k0, k1=k1: e.matmul(
                                    psum[:, bank, :], lhsT=wsl[slot][:, kc, j * 128:(j + 1) * 128], rhs=ytt[:, kc, :], start=(kc == k0), stop=(kc == k1 - 1)),
                                    reads=["wsl%d" % slot, "wgu%d" % (2 * slot), "wgu%d" % (2 * slot + 1), "ytt"], writes=["ps%d" % bank])
                        for b in range(3):
                            P.op("dve", lambda e, b=b, bank=banks[b], gi=gi: e.tensor_tensor(out=tm[b], in0=psum[:, bank, :], in1=gtl[gi][:, b, :], op=ALU.mult),
                                 reads=["ps%d" % banks[b], "gtl%d" % gi], writes=["tm%d" % b])
                        P.op("pool", lambda e: e.tensor_tensor(out=tm[0], in0=tm[0], in1=tm[1], op=ALU.add), reads=["tm0", "tm1"], writes=["tm0"])
                        P.op("pool", lambda e, nch=nch: e.tensor_tensor(out=mrg[:, nch, :], in0=tm[0], in1=tm[2], op=ALU.add), reads=["tm0", "tm2"], writes=["mrg"])
                for pn in range(D // 512 if PM >= 3 else 0):
                    slot = pn % 2
                    load_panel(wout, 0, KC, pn * 512, 512, wsl[slot], "wsl%d" % slot, "W_w_out", extra=["wgu%d" % (2 * slot), "wgu%d" % (2 * slot + 1)])
                    for j in range(4):
                        nch = pn * 4 + j
                        bank = nextbank()
                        for kc in range(KC):
                            P.op("pe", lambda e, kc=kc, bank=bank, slot=slot, j=j: e.matmul(psum[:, bank, :], lhsT=wsl[slot][:, kc, j * 128:(j + 1) * 128],
                                                                                       rhs=mrg[:, kc, :], start=(kc == 0), stop=(kc == KC - 1)),
                                 reads=["wsl%d" % slot, "wgu%d" % (2 * slot), "wgu%d" % (2 * slot + 1), "mrg"], writes=["ps%d" % bank])
                        P.op("dve", lambda e, bank=bank, nch=nch: e.tensor_tensor(out=xt3[:, nch, :], in0=psum[:, bank, :], in1=xt3[:, nch, :], op=ALU.add),
                             reads=["ps%d" % bank, "xt3"], writes=["xt3"])
                if PM >= 4:
                    rmsnorm_fm(xt3, "xt3", lambda kc, l=l: gffn[:, l, kc:kc + 1], 512, h2T, "h2T", sq3, "sq3", rstd3, "rstd3", psum[:, 7, :], "ps7",
                               h32=(h32 if is_moe else None), h32key="ytt")
                if is_moe and PM >= 5:
                    for tb in range(4):
                        for kc in range(KC):
                            P.op("pe", lambda e, kc=kc, tb=tb: e.matmul(psum[:, 6, 0:NE], lhsT=h32[:, kc, tb * 128:(tb + 1) * 128], rhs=wr_f[:, kc, :],
                                                                      start=(kc == 0), stop=(kc == KC - 1)),
                                 reads=["ytt", "wr_f"], writes=["ps6"])
                        P.op("dve", lambda e: e.tensor_copy(out=lg, in_=psum[:, 6, 0:NE]), reads=["ps6"], writes=["lg"])
                        P.op("dve", lambda e: e.max(out=m8, in_=lg), reads=["lg"], writes=["m8"])
                        P.op("dve", lambda e: e.tensor_tensor(out=gg[:, 0:1], in0=m8[:, 1:2], in1=m8[:, 0:1], op=ALU.subtract), reads=["m8"], writes=["gg"])
                        P.op("act", lambda e: e.activation(out=gg[:, 1:2], in_=gg[:, 0:1], func=AF.Exp), reads=["gg"], writes=["gg"])
                        P.op("dve", lambda e: e.tensor_scalar(out=gg[:, 2:3], in0=gg[:, 1:2], scalar1=1.0, scalar2=None, op0=ALU.add), reads=["gg"], writes=["gg"])
                        P.op("dve", lambda e: e.reciprocal(out=gg[:, 2:3], in_=gg[:, 2:3]), reads=["gg"], writes=["gg"])
                        P.op("dve", lambda e: e.tensor_tensor(out=gg[:, 3:4], in0=gg[:, 1:2], in1=gg[:, 2:3], op=ALU.mult), reads=["gg"], writes=["gg"])
                        P.op("dve", lambda e, tb=tb: e.tensor_scalar(out=wmat[:, tb, :], in0=lg, scalar1=m8[:, 0:1], scalar2=gg[:, 2:3], op0=ALU.is_equal, op1=ALU.mult),
                             reads=["lg", "m8", "gg"], writes=["wmat"])
                        P.op("dve", lambda e: e.tensor_scalar(out=w2, in0=lg, scalar1=m8[:, 1:2], scalar2=gg[:, 3:4], op0=ALU.is_equal, op1=ALU.mult),
                             reads=["lg", "m8", "gg"], writes=["w2"])
                        P.op("dve", lambda e, tb=tb: e.tensor_tensor(out=wmat[:, tb, :], in0=wmat[:, tb, :], in1=w2, op=ALU.add), reads=["wmat", "w2"], writes=["wmat"])
                nexp = NE if is_moe else 1
                for ex in range(nexp if PM >= 5 else 0):
                    if is_moe:
                        Wg = wfull["m_gate"][ex * D:(ex + 1) * D, :]
                        Wu = wfull["m_up"][ex * D:(ex + 1) * D, :]
                        Wd = wfull["m_down"][ex * EDIM:(ex + 1) * EDIM, :]
                        kg, ku, kd = "W_m_gate", "W_m_up", "W_m_down"
                        for tb in range(4):
                            P.op("dve", lambda e, tb=tb, ex=ex: e.tensor_scalar(out=wbc, in0=ones_f[:], scalar1=wmat[:, tb, ex:ex + 1], scalar2=None, op0=ALU.mult),
                                 reads=["wmat", "ones_f"], writes=["wbc"])
                            P.op("pe", lambda e, tb=tb: e.matmul(psum[:, 6, tb * 128:(tb + 1) * 128], lhsT=wbc, rhs=ident_f[:], start=True, stop=True),
                                 reads=["wbc", "ident_f"], writes=["ps6"])
                        P.op("act", lambda e: e.activation(out=wrep, in_=psum[:, 6, :], func=AF.Copy), reads=["ps6"], writes=["wrep"])
                    else:
                        Wg, Wu, Wd = wfull["f_gate"], wfull["f_up"], wfull["f_down"]
                        kg, ku, kd = "W_f_gate", "W_f_up", "W_f_down"
                        if os.environ.get("SRCSWAP"):
                            Wg, Wu = wfull["w_out"], wfull["w_out"]
                            kg, ku = "W_w_out", "W_w_out"
                    for fg in range(nff // FG):
                        f = 0
                        while f < FG:
                            nf = min(2, FG - f)
                            fcol = (fg * FG + f) * 128
                            gu_i["i"] += 1
                            sg_i = gu_i["i"] % 2
                            load_panel(Wg, 0, KC, fcol, nf * 128, wgu[sg_i], "wgu%d" % sg_i, kg)
                            load_panel(Wu, 0, KC, fcol, nf * 128, wgu[2 + sg_i], "wgu%d" % (2 + sg_i), ku)
                            for jj in range(nf if int(os.environ.get("FMODE", "9")) >= 1 else 0):
                                bg, bu = nextbank(), nextbank()
                                for kc in range(KC):
                                    P.op("pe", lambda e, kc=kc, bg=bg, sg_i=sg_i, jj=jj: e.matmul(psum[:, bg, :], lhsT=wgu[sg_i][:, kc, jj * 128:(jj + 1) * 128], rhs=h2T[:, kc, :],
                                                                                             start=(kc == 0), stop=(kc == KC - 1)),
                                         reads=["wgu%d" % sg_i, "h2T"], writes=["ps%d" % bg])
                                for kc in range(KC):
                                    P.op("pe", lambda e, kc=kc, bu=bu, sg_i=sg_i, jj=jj: e.matmul(psum[:, bu, :], lhsT=wgu[2 + sg_i][:, kc, jj * 128:(jj + 1) * 128], rhs=h2T[:, kc, :],
                                                                                             start=(kc == 0), stop=(kc == KC - 1)),
                                         reads=["wgu%d" % (2 + sg_i), "h2T"], writes=["ps%d" % bu])
                                si = (f + jj) % 2
                                FM = int(os.environ.get("FMODE", "9"))
                                if FM < 2:
                                    continue
                                P.op("act", lambda e, bg=bg, si=si: e.activation(out=sg[si], in_=psum[:, bg, :], func=AF.Sigmoid), reads=["ps%d" % bg], writes=["sg%d" % si])
                                P.op("dve", lambda e, bg=bg, si=si: e.tensor_tensor(out=sg[si], in0=psum[:, bg, :], in1=sg[si], op=ALU.mult),
                                     reads=["ps%d" % bg, "sg%d" % si], writes=["sg%d" % si])
                                if is_moe:
                                    P.op("dve", lambda e, bu=bu, si=si: e.tensor_tensor(out=sg[si], in0=psum[:, bu, :], in1=sg[si], op=ALU.mult),
                                         reads=["ps%d" % bu, "sg%d" % si], writes=["sg%d" % si])
                                    P.op("pool", lambda e, si=si, fi=f + jj: e.tensor_tensor(out=aT[:, fi, :], in0=sg[si], in1=wrep, op=ALU.mult),
                                         reads=["sg%d" % si, "wrep"], writes=["aT"])
                                else:
                                    P.op("dve", lambda e, bu=bu, si=si, fi=f + jj: e.tensor_tensor(out=aT[:, fi, :], in0=psum[:, bu, :], in1=sg[si], op=ALU.mult),
                                         reads=["ps%d" % bu, "sg%d" % si], writes=["aT"])
                            f += nf
                        for nch in range(KC if int(os.environ.get("FMODE", "9")) >= 3 else 0):
                            ds_ = nch % 2
                            r0 = fg * FG * 128
                            load_panel(Wd, r0, FG, nch * 128, 128, wdn[ds_], "wdn%d" % ds_, kd)
                            if int(os.environ.get("FMODE", "9")) < 4:
                                continue
                            bank = nextbank()
                            for fi in range(FG):
                                P.op("pe", lambda e, fi=fi, bank=bank, ds_=ds_: e.matmul(psum[:, bank, :], lhsT=wdn[ds_][:, fi, :], rhs=aT[:, fi, :], start=(fi == 0), stop=(fi == FG - 1)),
                                     reads=["wdn%d" % ds_, "aT"], writes=["ps%d" % bank])
                            P.op("dve", lambda e, bank=bank, nch=nch: e.tensor_tensor(out=xt3[:, nch, :], in0=psum[:, bank, :], in1=xt3[:, nch, :], op=ALU.add),
                                 reads=["ps%d" % bank, "xt3"], writes=["xt3"])
                if l == DEPTH - 1 and PM < 6:
                    P.dma("sp", lambda e, tsl=tsl: e.dma_start(out=out_d[:, tsl].rearrange("(k p) t -> p k t", p=128), in_=xt3), "outd", reads=["xt3"], writes=["OUT"])
                elif l == DEPTH - 1:
                    rmsnorm_fm(xt3, "xt3", lambda kc: gfin[:, kc:kc + 1], 512, outf, "ytt", sq3, "sq3", rstd3, "rstd3", psum[:, 7, :], "ps7")
                    P.dma("sp", lambda e, tsl=tsl: e.dma_start(out=out_d[:, tsl].rearrange("(k p) t -> p k t", p=128), in_=outf), "outd", reads=["ytt"], writes=["OUT"])
                else:
                    P.dma("sp", lambda e, tsl=tsl: e.dma_start(out=x_dst[:, tsl].rearrange("(k p) t -> p k t", p=128), in_=xt3), "xout", reads=["xt3"], writes=["X%d" % (l + 1)])
            P.barrier()
            return False

        x_src = xT_in
        for l in range(DEPTH):
            x_dst = xT_s[l % 2]
            if layer(l, x_src, x_dst):
                break
            x_src = x_dst
        if dbg:
            for nm, apname in dbg:
                src = {"QT_s": QT_s, "KT_all": KT_all, "V_all": V_all, "C_all": C_all, "yT_s": yT_s, "gT_s": gT_s,
                       "xT0": xT_s[0], "xT1": xT_s[1], "t5skew": t5skew}[apname]
                dd = nc.dram_tensor(nm, list(src.shape), src.dtype if hasattr(src, "dtype") else src.tensor.dtype, kind="ExternalOutput").ap()
                allk = sorted(set(list(P.last_w.keys())))
                P.dma("sp", lambda e, dd=dd, src=src: e.dma_start(out=dd, in_=src), "dbg", reads=allk, writes=["dbgout"])
        def fin(e):
            return e.nop() if hasattr(e, "nop") else e.engine_nop()
        allkeys = list(P.dma_count.keys())
        o = P.op("sp", fin, reads=[], writes=[])
        for eng in P.ENGS:
            lastk = {}
            for oo in P.ops[eng]:
                if oo.is_dma:
                    lastk[oo.key] = oo
            for k, oo in lastk.items():
                o.deps.append(oo)
        P.emit()
    return nc


def make_in_maps(cfg, inputs):
    D, S, TOK = cfg.D, cfg.SEQ, cfg.TOK
    f32 = np.float32
    x = np.asarray(inputs["x"], f32).reshape(S, D)
    w = {
        "w_in": np.asarray(inputs["w_in"], f32).reshape(-1, IN_WIDTH),
        "w_ba": np.asarray(inputs["w_branch_a"], f32).reshape(-1, D),
        "w_bb": np.asarray(inputs["w_branch_b"], f32).reshape(-1, D),
        "w_bc": np.asarray(inputs["w_branch_c"], f32).reshape(-1, D),
        "w_out": np.asarray(inputs["w_out"], f32).reshape(-1, D),
        "f_gate": np.asarray(inputs["ffn_gate"], f32).reshape(D, -1),
        "f_up": np.asarray(inputs["ffn_up"], f32).reshape(D, -1),
        "f_down": np.asarray(inputs["ffn_down"], f32).reshape(-1, D),
        "m_gate": np.asarray(inputs["moe_gate"], f32).reshape(-1, cfg.EDIM),
        "m_up": np.asarray(inputs["moe_up"], f32).reshape(-1, cfg.EDIM),
        "m_down": np.asarray(inputs["moe_down"], f32).reshape(-1, D),
    }
    t5 = np.asarray(inputs["t5_bias"], f32)
    rpb = np.asarray(inputs["na_rpb"], f32)
    rmat = np.zeros((128, 128), f32)
    for m in range(128):
        if m % 64 < 32:
            rmat[m + 32, m] = -1.0
        else:
            rmat[m - 32, m] = 1.0
    ident = np.eye(128, dtype=f32)
    maps = []
    for c in range(NCORES):
        cosT, sinT, t5idx, idxC, classes = host_tables(cfg, c)
        t5vec = np.zeros((4, TOK + S), f32)
        t5vec[:, :TOK + S - 1] = t5[t5idx].T
        RPC = TOK // cfg.GRID_W
        cls_rows = [0, 1, 2, 3, min(4, RPC - 1), RPC - 3, RPC - 2, RPC - 1]
        cb = np.stack([np.stack([na_bias_tile(cfg, rpb[l], *classes[rl]).reshape(128, -1) for rl in cls_rows]) for l in range(cfg.DEPTH)])
        m = {
            "xT": np.ascontiguousarray(x[c * TOK:(c + 1) * TOK].T),
            "norm_mix": np.asarray(inputs["norm_mix"], f32), "norm_ffn": np.asarray(inputs["norm_ffn"], f32),
            "norm_final": np.asarray(inputs["norm_final"], f32).reshape(1, D),
            "diff_lambda": np.asarray(inputs["diff_lambda"], f32).reshape(cfg.DEPTH, 256),
            "diff_subln": np.asarray(inputs["diff_subln"], f32),
            "qk_norm_b": np.asarray(inputs["qk_norm_b"], f32).reshape(cfg.DEPTH, 256),
            "moe_router": np.asarray(inputs["moe_router"], f32).reshape(D, cfg.NE),
            "cosT": cosT, "sinT": sinT, "rmat": rmat, "ident": ident, "t5vec": t5vec,
            "idxC": idxC, "cbias": np.ascontiguousarray(cb),
        }
        for nm, arr in w.items():
            Rr = arr.shape[0] // NCORES
            cols = arr.shape[1]
            nr = max(1, min(Rr, (512 * 1024) // (cols * 2)))
            parts = []
            b = 0
            while b < Rr:
                n = min(nr, Rr - b)
                parts.append(arr[NCORES * b + c * n:NCORES * b + (c + 1) * n])
                b += n
            m[nm] = np.ascontiguousarray(np.concatenate(parts, axis=0))
        maps.append(m)
    return maps


def kernel(**inputs):
    cfg = CFG
    nc = build_program(cfg)
    in_maps = make_in_maps(cfg, inputs)
    res = run_bass_kernel_spmd(nc, in_maps, core_ids=list(range(NCORES)))
    outs = [np.asarray(r["outT"]).T for r in res.results]
    return np.concatenate(outs, axis=0).reshape(1, cfg.SEQ, cfg.D).astype(np.float32)
```
